# Optimizing a Trainium2 kernel written in Bass

```python
import jax
import jax.numpy as jnp
from jax import lax
import numpy as np

D_MODEL = 1024
BATCH = 32
SEQ = 2048
DEPTH = 4

MEM_LEN = 256
NORM_EPS = 1e-6
D_FF = 2816
A_HEADS = 8
A_HEAD_DIM = 64
A_WIDTH = A_HEADS * A_HEAD_DIM
A_RANK_W = 64
A_RANK_A = 64
A_RANK_G = 128
A_LN_EPS = 64e-5
B_HEADS = 8
B_QK_DIM = 64
B_V_DIM = 128
B_QK_WIDTH = B_HEADS * B_QK_DIM
B_V_WIDTH = B_HEADS * B_V_DIM
B_CHUNK = 128
B_GN_EPS = 1e-5
ROPE_BASE = 10000.0
X_HEADS = 4
X_HEAD_DIM = D_MODEL // X_HEADS
A_SHIFT_SIZES = (A_WIDTH, A_WIDTH, A_WIDTH, A_RANK_W, A_RANK_A, A_RANK_G)
A_SHIFT_WIDTH = sum(A_SHIFT_SIZES)
B_SIZES = (B_QK_WIDTH, B_QK_WIDTH, B_V_WIDTH, B_V_WIDTH)
B_IN_WIDTH = sum(B_SIZES)
IN_WIDTH = A_SHIFT_WIDTH + B_IN_WIDTH + 2 * D_MODEL

kernel_name = "hybrid_rwkv7_retention_macaron"


def _split(z, sizes):
    return jnp.split(z, [int(s) for s in np.cumsum(sizes)[:-1]], axis=-1)


def rms_norm(x, g):
    xf = x.astype(jnp.float32)
    y = xf * lax.rsqrt(jnp.mean(xf * xf, axis=-1, keepdims=True) + NORM_EPS)
    return (y * g.astype(jnp.float32)).astype(x.dtype)


def _standardize(x, eps):
    mu = jnp.mean(x, axis=-1, keepdims=True)
    xc = x - mu
    return xc * lax.rsqrt(jnp.mean(xc * xc, axis=-1, keepdims=True) + eps)


def swiglu_ffn(x, w_in, w_out):
    gate, up = jnp.split(x @ w_in, 2, axis=-1)
    return (jax.nn.silu(gate) * up) @ w_out


def token_shift(z):
    return jnp.pad(z[:, :-1], ((0, 0), (1, 0), (0, 0)))


def rotary(x, positions):
    half = x.shape[-1] // 2
    inv_freq = ROPE_BASE ** (-jnp.arange(half, dtype=jnp.float32) / half)
    ang = positions.astype(jnp.float32)[:, :, None] * inv_freq
    cos = jnp.cos(ang)[:, :, None, :]
    sin = jnp.sin(ang)[:, :, None, :]
    x1, x2 = x[..., :half], x[..., half:]
    return jnp.concatenate([x1 * cos - x2 * sin, x1 * sin + x2 * cos], axis=-1)


def rwkv7_scan(r, decay, k, v, kk, a):
    B, T, H, N = r.shape

    def step(S, inp):
        r_t, w_t, k_t, v_t, kk_t, a_t = inp
        sa = jnp.einsum('bhij,bhj->bhi', S, -kk_t)
        S = (S * w_t[:, :, None, :]
             + sa[..., None] * (kk_t * a_t)[:, :, None, :]
             + v_t[..., None] * k_t[:, :, None, :])
        return S, jnp.einsum('bhij,bhj->bhi', S, r_t)

    xs = tuple(jnp.moveaxis(t, 1, 0) for t in (r, decay, k, v, kk, a))
    _, y = lax.scan(step, jnp.zeros((B, H, N, N), jnp.float32), xs)
    return jnp.moveaxis(y, 0, 1)


def rwkv7_time_mix(z_a, w0, w_up, a0, a_up, g_up, k_k, k_a, r_k, ln):
    B, T, _ = z_a.shape
    dtype = z_a.dtype
    f32 = jnp.float32
    r, k, v, w_lo, a_lo, g_lo = _split(z_a.astype(f32), A_SHIFT_SIZES)
    w = w0.astype(f32) + jnp.tanh(w_lo) @ w_up.astype(f32)
    decay = jnp.exp(-jnp.exp(-jax.nn.softplus(-w) - 0.5))
    a = jax.nn.sigmoid(a0.astype(f32) + a_lo @ a_up.astype(f32))
    g = jax.nn.sigmoid(g_lo) @ g_up.astype(f32)

    def heads(t):
        return t.reshape(B, T, A_HEADS, A_HEAD_DIM)

    kk = heads(k * k_k.astype(f32))
    kk = kk / jnp.maximum(jnp.sqrt(jnp.sum(kk * kk, axis=-1, keepdims=True)), 1e-12)
    k = k * (1.0 + (a - 1.0) * k_a.astype(f32))
    rh, kh, vh = heads(r), heads(k), heads(v)
    y = rwkv7_scan(rh, heads(decay), kh, vh, kk, heads(a))
    ln = ln.astype(f32)
    y = _standardize(y, A_LN_EPS).reshape(B, T, A_WIDTH) * ln[0] + ln[1]
    bonus = jnp.sum(rh * kh * r_k.astype(f32), axis=-1, keepdims=True) * vh
    y = y + bonus.reshape(B, T, A_WIDTH)
    return (y * g).astype(dtype)


def retention_chunkwise(q, k, v):
    B, T, H, dk = q.shape
    dv = v.shape[-1]
    C = B_CHUNK
    nc = T // C
    log_gamma = jnp.log1p(-jnp.exp2(-5.0 - jnp.arange(H, dtype=jnp.float32)))
    qc = q.reshape(B, nc, C, H, dk)
    kc = k.reshape(B, nc, C, H, dk)
    vc = v.reshape(B, nc, C, H, dv)
    pos = jnp.arange(C, dtype=jnp.float32)
    diff = pos[:, None] - pos[None, :]
    inner_decay = jnp.where(diff[None] >= 0,
                            jnp.exp(jnp.maximum(diff, 0.0)[None] * log_gamma[:, None, None]),
                            0.0)
    scores = jnp.einsum('bnchd,bnshd->bnhcs', qc, kc) * inner_decay[None, None]
    inner = jnp.einsum('bnhcs,bnshe->bnche', scores, vc)
    q_decay = jnp.exp((pos + 1.0)[None, :] * log_gamma[:, None])
    k_decay = jnp.exp((C - 1.0 - pos)[None, :] * log_gamma[:, None])
    chunk_decay = jnp.exp(C * log_gamma)
    kv = jnp.einsum('bnshd,bnshe->bnhde', kc * k_decay.T[None, None, :, :, None], vc)

    def step(R, kv_n):
        return R * chunk_decay[None, :, None, None] + kv_n, R

    _, r_prev = lax.scan(step, jnp.zeros((B, H, dk, dv), jnp.float32), jnp.moveaxis(kv, 1, 0))
    r_prev = jnp.moveaxis(r_prev, 0, 1)
    cross = jnp.einsum('bnchd,bnhde->bnche', qc * q_decay.T[None, None, :, :, None], r_prev)
    return (inner + cross).reshape(B, T, H, dv)


def retention_mix(z_b, positions, gn):
    B, T, _ = z_b.shape
    dtype = z_b.dtype
    q, k, v, g = _split(z_b.astype(jnp.float32), B_SIZES)
    qh = rotary(q.reshape(B, T, B_HEADS, B_QK_DIM), positions)
    kh = rotary(k.reshape(B, T, B_HEADS, B_QK_DIM), positions) * (B_QK_DIM ** -0.5)
    y = retention_chunkwise(qh, kh, v.reshape(B, T, B_HEADS, B_V_DIM))
    y = _standardize(y, B_GN_EPS).reshape(B, T, B_V_WIDTH) * gn.astype(jnp.float32)
    return (y * jax.nn.silu(g)).astype(dtype)


def memory_cross_attn(hn, mem, mem_g, w_q, w_kv, w_o):
    B, T, _ = hn.shape
    q = (hn @ w_q).reshape(B, T, X_HEADS, X_HEAD_DIM)
    k, v = jnp.split(rms_norm(mem, mem_g) @ w_kv, 2, axis=-1)
    k = k.reshape(B, MEM_LEN, X_HEADS, X_HEAD_DIM)
    v = v.reshape(B, MEM_LEN, X_HEADS, X_HEAD_DIM)
    s = jnp.einsum('bthd,bmhd->bhtm', q, k).astype(jnp.float32) * (X_HEAD_DIM ** -0.5)
    p = jax.nn.softmax(s, axis=-1).astype(v.dtype)
    o = jnp.einsum('bhtm,bmhd->bthd', p, v).reshape(B, T, D_MODEL)
    return o @ w_o


def setup_inputs(seed: int = 0) -> dict:
    key = jax.random.key(seed)
    ks = jax.random.split(key, 32)
    f32 = jnp.float32

    def nrm(k, shape, scale):
        return jax.random.normal(k, shape, f32) * scale

    x = nrm(ks[0], (BATCH, SEQ, D_MODEL), 1.0)
    mem = nrm(ks[1], (BATCH, MEM_LEN, D_MODEL), 1.0)
    offset = jax.random.randint(ks[2], (BATCH, 1), 0, 4096, dtype=jnp.int32)
    positions = offset + jnp.arange(SEQ, dtype=jnp.int32)[None, :]
    return {
        "x": x,
        "mem": mem,
        "positions": positions,
        "norm_g": 1.0 + nrm(ks[3], (DEPTH, 4, D_MODEL), 0.02),
        "ffn_w_in": nrm(ks[4], (DEPTH, 2, D_MODEL, 2 * D_FF), D_MODEL ** -0.5),
        "ffn_w_out": nrm(ks[5], (DEPTH, 2, D_FF, D_MODEL), 0.5 * D_FF ** -0.5),
        "mix_w_in": nrm(ks[6], (DEPTH, D_MODEL, IN_WIDTH), D_MODEL ** -0.5),
        "mix_gate_b": nrm(ks[7], (DEPTH, 2, D_MODEL), 0.02),
        "shift_mu": jax.random.uniform(ks[8], (DEPTH, A_SHIFT_WIDTH), f32),
        "a_w0": jax.random.uniform(ks[9], (DEPTH, A_WIDTH), f32, -6.0, -1.0),
        "a_w_up": nrm(ks[10], (DEPTH, A_RANK_W, A_WIDTH), 0.1 * A_RANK_W ** -0.5),
        "a_a0": nrm(ks[11], (DEPTH, A_WIDTH), 0.1),
        "a_a_up": nrm(ks[12], (DEPTH, A_RANK_A, A_WIDTH), 0.1 * A_RANK_A ** -0.5),
        "a_g_up": nrm(ks[13], (DEPTH, A_RANK_G, A_WIDTH), A_RANK_G ** -0.5),
        "a_k_k": 0.85 + nrm(ks[14], (DEPTH, A_WIDTH), 0.05),
        "a_k_a": 1.0 + nrm(ks[15], (DEPTH, A_WIDTH), 0.05),
        "a_r_k": nrm(ks[16], (DEPTH, A_HEADS, A_HEAD_DIM), 0.1),
        "a_ln": jnp.stack([1.0 + nrm(ks[17], (DEPTH, A_WIDTH), 0.02),
                           nrm(ks[18], (DEPTH, A_WIDTH), 0.02)], axis=1),
        "b_gn": 1.0 + nrm(ks[19], (DEPTH, B_V_WIDTH), 0.02),
        "w_branch_a": nrm(ks[20], (DEPTH, A_WIDTH, D_MODEL), A_WIDTH ** -0.5),
        "w_branch_b": nrm(ks[21], (DEPTH, B_V_WIDTH, D_MODEL), B_V_WIDTH ** -0.5),
        "mix_w_out": nrm(ks[22], (DEPTH, D_MODEL, D_MODEL), 0.5 * D_MODEL ** -0.5),
        "mem_norm": 1.0 + nrm(ks[23], (DEPTH, D_MODEL), 0.02),
        "cross_w_q": nrm(ks[24], (DEPTH, D_MODEL, D_MODEL), D_MODEL ** -0.5),
        "cross_w_kv": nrm(ks[25], (DEPTH, D_MODEL, 2 * D_MODEL), D_MODEL ** -0.5),
        "cross_w_o": nrm(ks[26], (DEPTH, D_MODEL, D_MODEL), 0.5 * D_MODEL ** -0.5),
        "final_norm": 1.0 + nrm(ks[27], (D_MODEL,), 0.02),
    }


def reference(x, mem, positions, norm_g, ffn_w_in, ffn_w_out, mix_w_in, mix_gate_b, shift_mu,
              a_w0, a_w_up, a_a0, a_a_up, a_g_up, a_k_k, a_k_a, a_r_k, a_ln, b_gn,
              w_branch_a, w_branch_b, mix_w_out, mem_norm, cross_w_q, cross_w_kv, cross_w_o,
              final_norm):
    h = x
    for l in range(DEPTH):
        h = h + 0.5 * swiglu_ffn(rms_norm(h, norm_g[l, 0]), ffn_w_in[l, 0], ffn_w_out[l, 0])
        u = rms_norm(h, norm_g[l, 1])
        z = u @ mix_w_in[l]
        z_a = z[..., :A_SHIFT_WIDTH]
        z_a = z_a + (token_shift(z_a) - z_a) * shift_mu[l]
        z_b = z[..., A_SHIFT_WIDTH:A_SHIFT_WIDTH + B_IN_WIDTH]
        g_pre = z[..., A_SHIFT_WIDTH + B_IN_WIDTH:]
        y_a = rwkv7_time_mix(z_a, a_w0[l], a_w_up[l], a_a0[l], a_a_up[l], a_g_up[l],
                             a_k_k[l], a_k_a[l], a_r_k[l], a_ln[l])
        y_b = retention_mix(z_b, positions, b_gn[l])
        gates = jax.nn.sigmoid(g_pre.astype(jnp.float32)
                               + mix_gate_b[l].reshape(-1).astype(jnp.float32)).astype(h.dtype)
        gate_a, gate_b = jnp.split(gates, 2, axis=-1)
        merged = gate_a * (y_a @ w_branch_a[l]) + gate_b * (y_b @ w_branch_b[l])
        h = h + merged @ mix_w_out[l]
        h = h + memory_cross_attn(rms_norm(h, norm_g[l, 2]), mem, mem_norm[l],
                                  cross_w_q[l], cross_w_kv[l], cross_w_o[l])
        h = h + 0.5 * swiglu_ffn(rms_norm(h, norm_g[l, 3]), ffn_w_in[l, 1], ffn_w_out[l, 1])
    return rms_norm(h, final_norm)
```

```python
import os
import numpy as np
import concourse.bass as bass
import concourse.mybir as mybir
from concourse.bass_utils import run_bass_kernel_spmd

F32 = mybir.dt.float32
BF16 = mybir.dt.bfloat16
I32 = mybir.dt.int32
AF = mybir.ActivationFunctionType
ALU = mybir.AluOpType

D = 1024
NCH = 8
DFF = 2816
NL = 4
MEM = 256
EPS = 1e-6
INW = 6912
C0 = float(np.exp(-0.5))


class Buf:
    __slots__ = ("name", "w", "r", "dsem", "dn")

    def __init__(self, name):
        self.name = name
        self.w = None
        self.r = {}
        self.dsem = None
        self.dn = 0


class Eng:
    def __init__(self, nc, name, h):
        self.name = name
        self.h = h
        self.sem = nc.alloc_semaphore("s_" + name)
        self.n = 0
        self.known = {}


class Builder:
    def __init__(self, T, NSEQ, layers, final=True, parts=("ffn1", "mix", "cross", "ffn2")):
        self.T = T
        self.NT = T // 512
        self.NSEQ = NSEQ
        self.layers = layers
        self.final = final
        self.parts = parts
        self.NLW = len(layers)
        self.lidx = {l: i for i, l in enumerate(layers)}
        nc = self.nc = bass.Bass("TRN2", target_bir_lowering=False)
        self.PE = Eng(nc, "pe", nc.tensor)
        self.ACT = Eng(nc, "act", nc.scalar)
        self.DVE = Eng(nc, "dve", nc.vector)
        self.POOL = Eng(nc, "pool", nc.gpsimd)
        self.SP = Eng(nc, "sp", nc.sync)
        self.engs = [self.PE, self.ACT, self.DVE, self.POOL, self.SP]
        self.dma_bufs = []
        self.dpool = {}
        base0 = nc.sbuf_base
        nbytes = (nc.sbuf_top - base0 - 256) // 64 * 64
        nc.alloc_sbuf_tensor("arena_all", [128, nbytes // 4], F32)
        self.sb_off = (base0 + 63) // 64 * 64
        self.sb_top = min(nc.sbuf_base, base0 + nbytes) // 64 * 64
        self.nalloc = 0
        self.heavy = False

    def alloc(self, shape, dt, at=None, name=None):
        esz = 2 if dt == BF16 else 4
        nbytes = int(np.prod(shape[1:])) * esz
        nbytes = (nbytes + 63) // 64 * 64
        if at is None:
            at = self.sb_off
            self.sb_off += nbytes
            assert self.sb_off <= self.sb_top, ("SBUF overflow", self.sb_off, self.sb_top)
        self.nalloc += 1
        t = self.nc.alloc_sbuf_tensor_at(f"{name or 't'}{self.nalloc}", list(shape), dt, offset=at)
        return t.ap()

    def _wait(self, eng, deps):
        for sem, val in deps:
            if eng is self.PE and sem is self.PE.sem:
                continue
            if eng.known.get(sem, 0) < val:
                eng.h.wait_ge(sem, val)
                eng.known[sem] = val

    def op(self, eng, fn, R=(), W=()):
        deps = []
        for b in R:
            if b.w is not None:
                deps.append(b.w)
        for b in W:
            if b.w is not None:
                deps.append(b.w)
            deps.extend(b.r.values())
        self._wait(eng, deps)
        ins = fn()
        eng.n += 1
        ins.then_inc(eng.sem, 1)
        tok = (eng.sem, eng.n)
        for b in R:
            b.r[eng.name] = tok
        for b in W:
            b.w = tok
            b.r = {}
        if self.heavy and eng is self.PE:
            self.PE.h.wait_ge(self.PE.sem, self.PE.n)

    def dma(self, q, pairs, buf, is_write):
        key = buf.name
        if key not in self.dpool:
            self.dpool[key] = [self.nc.alloc_semaphore("d_" + key), 0]
        ent = self.dpool[key]
        deps = []
        if buf.w is not None:
            deps.append(buf.w)
        if is_write:
            deps.extend(buf.r.values())
        self._wait(q, deps)
        for o, i in pairs:
            q.h.dma_start(out=o, in_=i).then_inc(ent[0], 16)
            ent[1] += 1
        tok = (ent[0], 16 * ent[1])
        if is_write:
            buf.w = tok
            buf.r = {}
        else:
            buf.r["dma"] = tok

    def barrier3(self):
        es = [self.PE, self.ACT, self.DVE]
        toks = [(e.sem, e.n) for e in es if e.n > 0]
        for e in es:
            self._wait(e, toks)

    def barrier(self):
        toks = [(e.sem, e.n) for e in self.engs if e.n > 0]
        toks += [(e[0], 16 * e[1]) for e in self.dpool.values() if e[1] > 0]
        for e in self.engs:
            self._wait(e, toks)

    def mm(self, out, lhsT, rhs, start=True, stop=True, R=(), W=()):
        self.op(self.PE, lambda: self.nc.tensor.matmul(out, lhsT, rhs, start=start, stop=stop), R, W)

    def tr(self, out, in_, R=(), W=()):
        n = in_.shape[0]
        self.op(self.PE, lambda: self.nc.tensor.transpose(out, in_, self.ident[0:n, 0:n]), R, W)

    def act(self, out, in_, func, R=(), W=(), **kw):
        self.op(self.ACT, lambda: self.nc.scalar.activation(out=out, in_=in_, func=func, **kw), R, W)

    def rsqrt_eps(self, dst, src, srcb, dstb, eps=EPS):
        self.op(self.DVE, lambda: self.nc.vector.tensor_scalar_add(dst, src, float(eps)), R=[srcb], W=[dstb])
        self.act(dst, dst, AF.Ln, W=[dstb])
        self.act(dst, dst, AF.Exp, W=[dstb], scale=-0.5)

    def bank(self):
        self.bank_i = (self.bank_i + 1) % len(self.rot)
        i = self.rot[self.bank_i]
        return self.ps[i], self.psb[i]

    def build(self):
        nc = self.nc
        T, NT, NSEQ = self.T, self.NT, self.NSEQ
        V = nc.vector
        dr = {}

        def din(name, shape, dt=F32):
            dr[name] = nc.dram_tensor(name, list(shape), dt, kind="ExternalInput").ap()
            return dr[name]

        x = din("x", [NSEQ, T, D])
        ffn_w_in = din("ffn_w_in", [self.NLW, 2, D, 2 * DFF])
        ffn_w_out = din("ffn_w_out", [self.NLW, 2, DFF, D])
        NLW = self.NLW
        mix_w_in = din("mix_w_in", [NLW, D, INW])
        w_branch_a = din("w_branch_a", [NLW, 512, D])
        w_branch_b = din("w_branch_b", [NLW, D, D])
        mix_w_out = din("mix_w_out", [NLW, D, D])
        cross_w_q = din("cross_w_q", [NLW, D, D])
        cross_w_kv = din("cross_w_kv", [NLW, D, 2 * D])
        cross_w_o = din("cross_w_o", [NLW, D, D])
        a_w_up = din("a_w_up", [NLW, 64, 512])
        a_a_up = din("a_a_up", [NLW, 64, 512])
        a_g_up = din("a_g_up", [NLW, 128, 512])
        mem = din("mem", [NSEQ, MEM, D])
        positions = din("positions", [NSEQ, T], I32)
        pvec = din("pvec", [128, self.NPV])
        cst = din("cst", [128, self.NCST])
        out = nc.dram_tensor("out", [NSEQ, T, D], F32, kind="ExternalOutput").ap()

        self.hT = self.alloc([128, NCH, T], F32, name="hT")
        self.hb = [[Buf(f"h{c}_{tt}") for tt in range(NT)] for c in range(NCH)]
        self.pv = self.alloc([128, self.NPV], F32, name="pv")
        self.pvb = Buf("pv")
        self.cs = self.alloc([128, self.NCST], F32, name="cs")
        self.csb = Buf("cs")
        self.ident = self.cs[:, self.CO["ident"]:self.CO["ident"] + 128]
        self.onesb = self.alloc([128, 128], BF16, name="onesb")
        self.onesbb = Buf("onesb")
        self.ps = [nc.alloc_psum_tensor(f"ps{i}", [128, 512], F32).ap() for i in range(8)]
        self.psb = [Buf(f"ps{i}") for i in range(8)]
        self.bank_i = 0
        self.rot = list(range(8))
        sq = [self.alloc([128, 512], BF16, name="sq") for _ in range(2)]
        sqb = [Buf(f"sq{i}") for i in range(2)]
        rstd = self.alloc([128, 512], F32, name="rstd")
        rstdb = Buf("rstd")
        arena0 = self.sb_off

        self.sb_off = arena0
        xn = self.alloc([128, NT, NCH, 512], BF16, name="xn")
        xnb = [Buf(f"xn{tt}") for tt in range(NT)]
        wA = [self.alloc([128, 8, 1024], BF16, name="wA") for _ in range(2)]
        wAb = [Buf(f"wA{i}") for i in range(2)]
        wB = [self.alloc([128, 4, 1024], BF16, name="wB") for _ in range(2)]
        wBb = [Buf(f"wB{i}") for i in range(2)]
        actt = [self.alloc([128, 4, 512], BF16, name="act") for _ in range(2)]
        actb = [Buf(f"act{i}") for i in range(2)]
        stmp = [self.alloc([128, 512], F32, name="stmp") for _ in range(2)]
        stmpb = [Buf(f"stmp{i}") for i in range(2)]
        stage = self.alloc([128, 1024], F32, name="stage")
        stageb = Buf("stage")
        ffn_end = self.sb_off

        self.dma(self.SP, [(self.pv, pvec)], self.pvb, True)
        self.dma(self.SP, [(self.cs, cst)], self.csb, True)
        self.op(self.DVE, lambda: V.memset(self.onesb, 1.0 / 1024.0), W=[self.onesbb])

        sqi = [0]

        def rmsnorm_tile(tt, gcol, dst_fn, dstbufs):
            pb, pbb = self.bank()
            for c in range(NCH):
                i = sqi[0] % 2
                sqi[0] += 1
                self.act(sq[i], self.hT[:, c, tt * 512:(tt + 1) * 512], AF.Square, R=[self.hb[c][tt]], W=[sqb[i]])
                self.mm(pb, self.onesb, sq[i], start=(c == 0), stop=(c == NCH - 1), R=[self.onesbb, sqb[i]], W=[pbb])
            self.rsqrt_eps(rstd, pb, pbb, rstdb)
            for c in range(NCH):
                self.op(self.DVE, lambda c=c: V.scalar_tensor_tensor(
                    dst_fn(c), self.hT[:, c, tt * 512:(tt + 1) * 512], self.pv[:, gcol + c:gcol + c + 1], rstd,
                    ALU.mult, ALU.mult), R=[self.hb[c][tt], self.pvb, rstdb], W=dstbufs)

        def ffn(l, f):
            gcol = self.PO["norm_g"] + (l * 4 + (0 if f == 0 else 3)) * NCH
            for tt in range(NT):
                rmsnorm_tile(tt, gcol, lambda c, tt=tt: xn[:, tt, c, :], [xnb[tt]])
            nslab = 6
            wi = ffn_w_in[self.lidx[l], f]
            wo = ffn_w_out[self.lidx[l], f]

            def load(s):
                gs = 512 if s < 5 else 256
                a = s % 2
                self.dma(self.POOL, [(wA[a][:, k, o:o + gs], wi[k * 128:(k + 1) * 128, c0:c0 + gs])
                                     for k in range(NCH) for (o, c0) in ((0, s * 512), (512, DFF + s * 512))], wAb[a], True)
                self.dma(self.POOL, [(wB[a][:, k, :], wo[s * 512 + k * 128:s * 512 + (k + 1) * 128, :])
                                     for k in range(gs // 128)], wBb[a], True)

            load(0)
            ai = 0
            for s in range(nslab):
                if s + 1 < nslab:
                    load(s + 1)
                gs = 512 if s < 5 else 256
                nk = gs // 128
                a = s % 2
                for tt in range(NT):
                    at_ = actt[ai % 2]
                    atb = actb[ai % 2]
                    ai += 1
                    for j in range(nk):
                        pg, pgb = self.bank()
                        pu, pub = self.bank()
                        for k in range(NCH):
                            self.mm(pg, wA[a][:, k, j * 128:(j + 1) * 128], xn[:, tt, k, :], start=(k == 0), stop=(k == NCH - 1),
                                    R=[wAb[a], xnb[tt]], W=[pgb])
                        for k in range(NCH):
                            self.mm(pu, wA[a][:, k, 512 + j * 128:512 + (j + 1) * 128], xn[:, tt, k, :], start=(k == 0), stop=(k == NCH - 1),
                                    R=[wAb[a], xnb[tt]], W=[pub])
                        si = (ai * 4 + j) % 2
                        self.act(stmp[si], pg, AF.Silu, R=[pgb], W=[stmpb[si]])
                        self.op(self.DVE, lambda j=j, si=si, pu=pu, at_=at_: V.tensor_tensor(at_[:, j, :], stmp[si], pu, ALU.mult),
                                R=[stmpb[si], pub], W=[atb])
                    for d in range(NCH):
                        po, pob = self.bank()
                        for j in range(nk):
                            self.mm(po, wB[a][:, j, d * 128:(d + 1) * 128], at_[:, j, :], start=(j == 0), stop=(j == nk - 1),
                                    R=[wBb[a], atb], W=[pob])
                        hsl = self.hT[:, d, tt * 512:(tt + 1) * 512]
                        self.op(self.DVE, lambda po=po, hsl=hsl: V.scalar_tensor_tensor(hsl, po, 0.5, hsl, ALU.mult, ALU.add),
                                R=[pob], W=[self.hb[d][tt]])

        def wload(slot, slotb, W2, r0, nk, c0, ncols, o0=0, first=True):
            self.dma(self.POOL, [(slot[:, k, o0:o0 + ncols], W2[r0 + k * 128:r0 + (k + 1) * 128, c0:c0 + ncols])
                                 for k in range(nk)], slotb, True)

        def cross(s, l):
            li = self.lidx[l]
            self.sb_off = arena0
            xq = self.alloc([128, NCH, 512], BF16); xqb = Buf("xq")
            wS = [self.alloc([128, 8, 1024], BF16) for _ in range(2)]; wSb = [Buf("wS0"), Buf("wS1")]
            mstage = self.alloc([128, 1024], F32); mstb = Buf("mst")
            junk = self.alloc([128, 1024], F32); junkb = Buf("junk")
            mh = self.alloc([128, 8, 256], BF16); mhb = Buf("mh")
            KT = self.alloc([128, 8, 256], BF16); KTb = Buf("KT")
            Vt = self.alloc([128, 2, 1024], BF16); Vtb = Buf("Vt")
            qT = self.alloc([128, 8, 512], BF16); qTb = Buf("qT")
            oT = self.alloc([128, 8, 512], BF16); oTb = Buf("oT")
            pp = [self.alloc([128, 256], F32) for _ in range(2)]; ppb = [Buf("p0"), Buf("p1")]
            pn = [self.alloc([128, 256], F32) for _ in range(2)]; pnb = [Buf("pn0"), Buf("pn1")]
            pT = [self.alloc([128, 2, 512], BF16) for _ in range(2)]; pTb = [Buf("pT0"), Buf("pT1")]
            stt = [self.alloc([128, 8], F32) for _ in range(2)]; sttb = [Buf("st0"), Buf("st1")]
            wkv = cross_w_kv[li]
            wload(wS[0], wSb[0], wkv, 0, 8, 0, 1024)
            wload(wS[1], wSb[1], wkv, 0, 8, 1024, 1024)
            mcol = self.PO["mem_norm"] + l * NCH
            for mb in range(2):
                self.dma(self.SP, [(mstage, mem[s, mb * 128:(mb + 1) * 128, :])], mstb, True)
                st = stt[0]; stb = sttb[0]
                self.act(junk, mstage, AF.Square, R=[mstb], W=[junkb, stb], accum_out=st[:, 0:1])
                self.op(self.DVE, lambda: V.tensor_scalar(st[:, 1:2], st[:, 0:1], 1.0 / 1024.0, EPS, ALU.mult, ALU.add), W=[stb])
                self.act(st[:, 1:2], st[:, 1:2], AF.Ln, W=[stb])
                self.act(st[:, 1:2], st[:, 1:2], AF.Exp, W=[stb], scale=-0.5)
                self.op(self.DVE, lambda: V.tensor_scalar_mul(mstage, mstage, st[:, 1:2]), R=[stb], W=[mstb])
                for half in range(2):
                    pb, pbb = self.bank()
                    for cc in range(4):
                        c = half * 4 + cc
                        self.tr(pb[:, cc * 128:(cc + 1) * 128], mstage[:, c * 128:(c + 1) * 128], R=[mstb, self.csb], W=[pbb])
                    for cc in range(4):
                        c = half * 4 + cc
                        self.act(mh[:, c, mb * 128:(mb + 1) * 128], pb[:, cc * 128:(cc + 1) * 128], AF.Copy, R=[pbb, self.pvb], W=[mhb],
                                 scale=self.pv[:, mcol + c:mcol + c + 1])
            for dc in range(NCH):
                pb, pbb = self.bank()
                for k in range(NCH):
                    self.mm(pb[:, 0:256], wS[0][:, k, dc * 128:(dc + 1) * 128], mh[:, k, :], start=(k == 0), stop=(k == 7), R=[wSb[0], mhb], W=[pbb])
                self.act(KT[:, dc, :], pb[:, 0:256], AF.Copy, R=[pbb], W=[KTb])
            for mb in range(2):
                for half in range(2):
                    pb, pbb = self.bank()
                    for k in range(NCH):
                        self.mm(pb, mh[:, k, mb * 128:(mb + 1) * 128], wS[1][:, k, half * 512:(half + 1) * 512], start=(k == 0), stop=(k == 7),
                                R=[wSb[1], mhb], W=[pbb])
                    self.op(self.DVE, lambda pb=pb: V.tensor_copy(Vt[:, mb, half * 512:(half + 1) * 512], pb), R=[pbb], W=[Vtb])
            wload(wS[0], wSb[0], cross_w_q[li], 0, 8, 0, 1024)
            wload(wS[1], wSb[1], cross_w_o[li], 0, 8, 0, 1024)
            gcol = self.PO["norm_g"] + (l * 4 + 2) * NCH
            it = 0
            for tt in range(NT):
                rmsnorm_tile(tt, gcol, lambda c: xq[:, c, :], [xqb])
                for dc in range(NCH):
                    pb, pbb = self.bank()
                    for k in range(NCH):
                        self.mm(pb, wS[0][:, k, dc * 128:(dc + 1) * 128], xq[:, k, :], start=(k == 0), stop=(k == 7), R=[wSb[0], xqb], W=[pbb])
                    self.act(qT[:, dc, :], pb, AF.Copy, R=[pbb], W=[qTb], scale=1.0 / 16.0)
                for hd in range(4):
                    pt = pT[hd % 2]; ptb = pTb[hd % 2]
                    for tb in range(4):
                        i2 = it % 2; it += 1
                        st = stt[i2]; stb = sttb[i2]
                        pb, pbb = self.bank()
                        for kk in range(2):
                            self.mm(pb[:, 0:256], qT[:, 2 * hd + kk, tb * 128:(tb + 1) * 128], KT[:, 2 * hd + kk, :], start=(kk == 0), stop=(kk == 1),
                                    R=[qTb, KTb], W=[pbb])
                        self.op(self.DVE, lambda pb=pb, st=st: V.tensor_reduce(st[:, 0:1], pb[:, 0:256], mybir.AxisListType.X, ALU.max), R=[pbb], W=[stb])
                        self.op(self.DVE, lambda st=st: V.tensor_scalar_mul(st[:, 1:2], st[:, 0:1], -1.0), W=[stb])
                        self.act(pp[i2], pb[:, 0:256], AF.Exp, R=[pbb], W=[ppb[i2], stb], bias=st[:, 1:2], accum_out=st[:, 2:3])
                        self.op(self.DVE, lambda st=st: V.reciprocal(st[:, 3:4], st[:, 2:3]), W=[stb])
                        self.op(self.DVE, lambda st=st, i2=i2: V.tensor_scalar_mul(pn[i2], pp[i2], st[:, 3:4]), R=[ppb[i2], stb], W=[pnb[i2]])
                        pb2, pbb2 = self.bank()
                        for mb in range(2):
                            self.tr(pb2[:, mb * 128:(mb + 1) * 128], pn[i2][:, mb * 128:(mb + 1) * 128], R=[pnb[i2], self.csb], W=[pbb2])
                        self.act(pt[:, :, tb * 128:(tb + 1) * 128], pb2[:, 0:256].rearrange("p (m t) -> p m t", m=2), AF.Copy, R=[pbb2], W=[ptb])
                    for cc in range(2):
                        pb, pbb = self.bank()
                        for mb in range(2):
                            self.mm(pb, Vt[:, mb, hd * 256 + cc * 128:hd * 256 + (cc + 1) * 128], pt[:, mb, :], start=(mb == 0), stop=(mb == 1),
                                    R=[Vtb, ptb], W=[pbb])
                        self.op(self.DVE, lambda pb=pb, hd=hd, cc=cc: V.tensor_copy(oT[:, 2 * hd + cc, :], pb), R=[pbb], W=[oTb])
                for d in range(NCH):
                    pb, pbb = self.bank()
                    for k in range(NCH):
                        self.mm(pb, wS[1][:, k, d * 128:(d + 1) * 128], oT[:, k, :], start=(k == 0), stop=(k == 7), R=[wSb[1], oTb], W=[pbb])
                    hsl = self.hT[:, d, tt * 512:(tt + 1) * 512]
                    self.op(self.DVE, lambda pb=pb, hsl=hsl: V.tensor_tensor(hsl, hsl, pb, ALU.add), R=[pbb], W=[self.hb[d][tt]])

        def mixer(s, l):
            SK = set(os.environ.get('KSKIP', '').split(','))
            li = self.lidx[l]
            X_ = mybir.AxisListType.X
            self.sb_off = arena0
            CO = self.CO
            cs = self.cs
            csb = self.csb
            pvb = self.pvb
            pv = self.pv

            def A(shape, dt=F32):
                return self.alloc(shape, dt)
            carry = A([128, 16]); carryb = Buf("carry")
            Pst = A([128, 4, 64]); Pstb = [[Buf(f"P{h}{e}") for e in range(2)] for h in range(4)]
            Rst = A([128, 4, 128]); Rstb = [[Buf(f"R{h}{e}") for e in range(2)] for h in range(4)]
            Rbf = A([128, 4, 128], BF16)
            ut = A([128, 8, 512], BF16); utb = Buf("ut")
            wS = [A([128, 8, 1024], BF16) for _ in range(2)]; wSb = [Buf("wS0"), Buf("wS1")]
            ya = A([128, 4, 512], BF16); yab = Buf("ya")
            yb = A([128, 8, 512], BF16); ybb = Buf("yb")
            self.dbg = {'ya': ya.name, 'yb': yb.name, 'ut': ut.name}
            lrw = A([128, 512]); lrwb = Buf("lrw")
            gup = A([128, 512]); gupb = Buf("gup")
            scr0 = self.sb_off
            mw = mix_w_in[li]
            self.dma(self.SP, [(lrw[0:64, :], a_w_up[li]), (lrw[64:128, :], a_a_up[li])], lrwb, True)
            self.dma(self.SP, [(gup, a_g_up[li])], gupb, True)
            self.op(self.DVE, lambda: V.memset(carry, 0.0), W=[carryb])
            self.op(self.DVE, lambda: V.memset(Pst, 0.0), W=[b for r in Pstb for b in r])
            self.op(self.DVE, lambda: V.memset(Rst, 0.0), W=[b for r in Rstb for b in r])
            self.op(self.DVE, lambda: V.memset(Rbf, 0.0), W=[b for r in Rstb for b in r])
            D_ = self.DVE

            def tt_(o, a, b, op, R, W):
                self.op(D_, lambda: V.tensor_tensor(o, a, b, op), R, W)

            def ts_(o, a, s1, s2, op0, op1, R, W):
                self.op(D_, lambda: V.tensor_scalar(o, a, s1, s2, op0, op1), R, W)

            def stt_(o, a, sc, b, op0, op1, R, W):
                self.op(D_, lambda: V.scalar_tensor_tensor(o, a, sc, b, op0, op1), R, W)

            def cp_(o, a, R, W):
                self.op(D_, lambda: V.tensor_copy(o, a), R, W)
            UA = os.environ.get('KUA', 'act')

            def cpA(o, a, R, W):
                if UA == 'act':
                    self.act(o, a, AF.Copy, R=R, W=W)
                else:
                    cp_(o, a, R, W)
            ident = self.ident
            bo1 = cs[:, CO["bo1"]:CO["bo1"] + 128]
            bo64 = cs[:, CO["bo64"]:CO["bo64"] + 128]
            on128 = cs[:, CO["on128"]:CO["on128"] + 128]
            m_sl = cs[:, CO["m_sl"]:CO["m_sl"] + 128]
            m_up2 = cs[:, CO["m_up2"]:CO["m_up2"] + 256]
            resetm = cs[:, CO["reset"]:CO["reset"] + 512]
            gcolu = self.PO["norm_g"] + (l * 4 + 1) * NCH
            gam = [1.0 - 2.0 ** (-5.0 - h) for h in range(8)]

            def gnorm(src, srcb, ones_m, eps, t1, t1b, t2, t2b):
                pm1, pm1b = self.bank()
                self.mm(pm1, ones_m, src, R=[csb, srcb], W=[pm1b])
                self.act(t1, src, AF.Square, R=[srcb], W=[t1b])
                pm2, pm2b = self.bank()
                self.mm(pm2, ones_m, t1, R=[csb, t1b], W=[pm2b])
                self.act(t1, pm1, AF.Copy, R=[pm1b], W=[t1b])
                tt_(t2, t1, t1, ALU.mult, [t1b], [t2b])
                tt_(t2, pm2, t2, ALU.subtract, [pm2b], [t2b])
                self.rsqrt_eps(t2, t2, t2b, t2b, eps=eps)
                tt_(src, src, t1, ALU.subtract, [t1b], [srcb])
                tt_(src, src, t2, ALU.mult, [t2b], [srcb])

            for tt in range(NT):
                tsl = slice(tt * 512, (tt + 1) * 512)
                rmsnorm_tile(tt, gcolu, lambda c: ut[:, c, :], [utb])
                self.sb_off = scr0
                zb = A([128, 5, 516]); zbb = [Buf(f"z{i}") for i in range(5)]
                names = ["sg", "al", "gg", "cs_", "csm", "dcs", "E0", "E1", "kkn", "tmp", "kp",
                         "At", "Bt", "Kt", "Rt", "Bh", "Kh", "bon", "lo12", "lo13"]
                S_ = {n: A([128, 512]) for n in names}
                Sb = {n: Buf(n) for n in names}
                for a_, b_ in (("dd", "tmp"), ("inv", "E0"), ("ba", "sg"), ("yA", "kkn"), ("vsa", "csm")):
                    S_[a_] = S_[b_]
                    Sb[a_] = Sb[b_]
                EC = A([128, 8]); ECb = Buf("EC")
                TM = [S_["E0"], S_["E1"]]; TMb_ = [Sb["E0"], Sb["E1"]]
                Lm = A([128, 128]); Lmb = Buf("Lm")
                NMr = A([128, 256]); NMrb = Buf("NMr")
                KMr = A([128, 256]); KMrb = Buf("KMr")
                LN = [A([128, 256]) for _ in range(2)]; LNb = [Buf("LN0"), Buf("LN1")]
                Qm = [A([128, 128]) for _ in range(2)]; Qmb = [Buf("Q0"), Buf("Q1")]
                MVs = A([128, 64]); MVsb = Buf("MVs")
                WX = A([128, 128]); WXb = Buf("WX")
                GT = A([128, 128]); GTb = Buf("GT")
                YlT = A([128, 128]); YlTb = Buf("YlT")
                FTs = A([128, 64]); FTsb = Buf("FTs")
                pev = [S_["tmp"][:, 0:128], S_["tmp"][:, 128:256]]; pevb = [Sb["tmp"], Sb["tmp"]]
                pei = [0]

                def padd(o, ps_ap, sb_ap, R, W, rw_):
                    i = pei[0] % 2; pei[0] += 1
                    n = ps_ap.shape[-1]
                    self.act(pev[i][rw_, 0:n], ps_ap, AF.Copy, R=R, W=[pevb[i]])
                    tt_(o, sb_ap, pev[i][rw_, 0:n], ALU.add, [pevb[i]] + list(R), W)
                Hs = A([128, 64]); Hsb = Buf("Hs")
                wload(wS[0], wSb[0], mw, 0, 8, 0, 1024)
                wload(wS[1], wSb[1], mw, 0, 8, 1024, 768)

                def projA(ci, zi):
                    slot = 0 if ci < 8 else 1
                    col = ci * 128 - (0 if ci < 8 else 1024)
                    pb, pbb = self.bank()
                    for k in range(NCH):
                        self.mm(pb, wS[slot][:, k, col:col + 128], ut[:, k, :], start=(k == 0), stop=(k == 7), R=[wSb[slot], utb], W=[pbb])
                    cp_(zb[:, zi, 0:1], carry[:, ci:ci + 1], [carryb], [zbb[zi]])
                    self.act(zb[:, zi, 1:513], pb, AF.Copy, R=[pbb], W=[zbb[zi]])
                    cp_(carry[:, ci:ci + 1], zb[:, zi, 512:513], [zbb[zi]], [carryb])
                    tt_(S_["dd"], zb[:, zi, 0:512], zb[:, zi, 1:513], ALU.subtract, [zbb[zi]], [Sb["dd"]])
                    mu = pv[:, self.PO["shift_mu"] + l * 14 + ci:self.PO["shift_mu"] + l * 14 + ci + 1]
                    stt_(zb[:, zi, 1:513], S_["dd"], mu, zb[:, zi, 1:513], ALU.mult, ALU.add, [Sb["dd"], pvb], [zbb[zi]])

                projA(12, 3)
                projA(13, 4)
                self.act(S_["lo12"][0:64, :], zb[0:64, 3, 1:513], AF.Tanh, R=[zbb[3]], W=[Sb["lo12"]])
                self.act(S_["lo12"][64:128, :], zb[64:128, 3, 1:513], AF.Copy, R=[zbb[3]], W=[Sb["lo12"]])
                self.act(S_["lo13"], zb[:, 4, 1:513], AF.Sigmoid, R=[zbb[4]], W=[Sb["lo13"]])
                for hp in range(0 if 'rwkv' in SK else 4):
                    hc = slice(hp * 128, (hp + 1) * 128)
                    projA(hp, 0)
                    projA(4 + hp, 1)
                    projA(8 + hp, 2)
                    rs_, ks_, vs_ = zb[:, 0, 1:513], zb[:, 1, 1:513], zb[:, 2, 1:513]
                    pcol = lambda nm: pv[:, self.PO[nm] + l * 4 + hp:self.PO[nm] + l * 4 + hp + 1]
                    pw, pwb = self.bank()
                    self.mm(pw, lrw[0:64, hc], S_["lo12"][0:64, :], R=[lrwb, Sb["lo12"]], W=[pwb])
                    self.act(S_["sg"], pw, AF.Sigmoid, R=[pwb, pvb], W=[Sb["sg"]], bias=pcol("w0"))
                    pa, pab = self.bank()
                    self.mm(pa, lrw[64:128, hc], S_["lo12"][64:128, :], R=[lrwb, Sb["lo12"]], W=[pab])
                    self.act(S_["al"], pa, AF.Sigmoid, R=[pab, pvb], W=[Sb["al"]], bias=pcol("a0"))
                    pg, pgb = self.bank()
                    self.mm(pg, gup[:, hc], S_["lo13"], R=[gupb, Sb["lo13"]], W=[pgb])
                    self.act(S_["gg"], pg, AF.Copy, R=[pgb], W=[Sb["gg"]])
                    self.op(D_, lambda: V.tensor_tensor_scan(S_["cs_"], resetm, S_["sg"], 0.0, ALU.mult, ALU.add), R=[csb, Sb["sg"]], W=[Sb["cs_"]])
                    tt_(S_["csm"], S_["cs_"], S_["sg"], ALU.subtract, [Sb["cs_"], Sb["sg"]], [Sb["csm"]])
                    for c in range(8):
                        ts_(S_["dcs"][:, c * 64:(c + 1) * 64], S_["cs_"][:, c * 64:(c + 1) * 64], S_["cs_"][:, c * 64 + 63:c * 64 + 64], -1.0,
                            ALU.subtract, ALU.mult, [Sb["cs_"]], [Sb["dcs"]])
                    self.act(EC, S_["cs_"][:, 63:512:64], AF.Exp, R=[Sb["cs_"]], W=[ECb], scale=-C0)
                    self.op(D_, lambda: V.tensor_scalar_mul(S_["kkn"], ks_, pcol("kk")), R=[zbb[1], pvb], W=[Sb["kkn"]])
                    tt_(S_["tmp"], S_["kkn"], S_["kkn"], ALU.mult, [Sb["kkn"]], [Sb["tmp"]])
                    pq, pqb = self.bank()
                    self.mm(pq, bo1, S_["tmp"], R=[csb, Sb["tmp"]], W=[pqb])
                    self.op(D_, lambda: V.tensor_scalar_max(S_["inv"], pq, 1e-24), R=[pqb], W=[Sb["inv"]])
                    self.act(S_["inv"], S_["inv"], AF.Ln, W=[Sb["inv"]])
                    self.act(S_["inv"], S_["inv"], AF.Exp, W=[Sb["inv"]], scale=-0.5)
                    tt_(S_["kkn"], S_["kkn"], S_["inv"], ALU.mult, [Sb["inv"]], [Sb["kkn"]])
                    ts_(S_["tmp"], S_["al"], -1.0, pcol("ka"), ALU.add, ALU.mult, [Sb["al"], pvb], [Sb["tmp"]])
                    stt_(S_["kp"], S_["tmp"], 1.0, ks_, ALU.add, ALU.mult, [Sb["tmp"], zbb[1]], [Sb["kp"]])
                    stt_(S_["tmp"], rs_, pcol("rk"), S_["kp"], ALU.mult, ALU.mult, [zbb[0], pvb, Sb["kp"]], [Sb["tmp"]])
                    pbn, pbnb = self.bank()
                    self.mm(pbn, bo1, S_["tmp"], R=[csb, Sb["tmp"]], W=[pbnb])
                    tt_(S_["bon"], pbn, vs_, ALU.mult, [pbnb, zbb[2]], [Sb["bon"]])
                    tt_(S_["ba"], S_["kkn"], S_["al"], ALU.mult, [Sb["kkn"], Sb["al"]], [Sb["ba"]])
                    self.act(S_["E0"], S_["csm"], AF.Exp, R=[Sb["csm"]], W=[Sb["E0"]], scale=-C0)
                    stt_(S_["At"], S_["kkn"], -1.0, S_["E0"], ALU.mult, ALU.mult, [Sb["kkn"], Sb["E0"]], [Sb["At"]])
                    self.act(S_["E1"], S_["cs_"], AF.Exp, R=[Sb["cs_"]], W=[Sb["E1"]], scale=C0)
                    tt_(S_["Bt"], S_["ba"], S_["E1"], ALU.mult, [Sb["ba"], Sb["E1"]], [Sb["Bt"]])
                    tt_(S_["Kt"], S_["kp"], S_["E1"], ALU.mult, [Sb["kp"], Sb["E1"]], [Sb["Kt"]])
                    self.act(S_["E0"], S_["cs_"], AF.Exp, R=[Sb["cs_"]], W=[Sb["E0"]], scale=-C0)
                    tt_(S_["Rt"], rs_, S_["E0"], ALU.mult, [zbb[0], Sb["E0"]], [Sb["Rt"]])
                    self.act(S_["E1"], S_["dcs"], AF.Exp, R=[Sb["dcs"]], W=[Sb["E1"]], scale=-C0)
                    tt_(S_["Bh"], S_["ba"], S_["E1"], ALU.mult, [Sb["ba"], Sb["E1"]], [Sb["Bh"]])
                    tt_(S_["Kh"], S_["kp"], S_["E1"], ALU.mult, [Sb["kp"], Sb["E1"]], [Sb["Kh"]])
                    At, Bt, Kt, Rt = S_["At"], S_["Bt"], S_["Kt"], S_["Rt"]
                    self.act(S_["vsa"], vs_, AF.Copy, R=[zbb[2]], W=[Sb["vsa"]])
                    for b in range(0 if 'units' in SK else 4):
                        cols = slice(b * 128, (b + 1) * 128)
                        tm = TM[b % 2]; tmb = TMb_[b % 2]
                        pb, pbb = self.bank()
                        for qi, (src, sb_) in enumerate([(At, Sb["At"]), (S_["Bh"], Sb["Bh"]), (S_["Kh"], Sb["Kh"]), (S_["vsa"], Sb["vsa"])]):
                            self.tr(pb[:, qi * 128:(qi + 1) * 128], src[:, cols], R=[sb_, csb], W=[pbb])
                        cpA(tm, pb, [pbb], [tmb])
                        for e in range(2):
                            self.heavy = 'noheavy' not in SK
                            r0 = e * 64
                            rw = slice(r0, r0 + 64)
                            A_tm = tm[:, r0:r0 + 64]
                            Bh_tm = tm[:, 128 + r0:128 + r0 + 64]
                            Kh_tm = tm[:, 256 + r0:256 + r0 + 64]
                            V_tm = tm[:, 384 + r0:384 + r0 + 64]
                            RA = [Sb["At"], Sb["Bt"], Sb["Kt"], Sb["Rt"]]
                            p1, p1b = self.bank()
                            self.mm(p1[:, 0:128], At[rw, cols], Bt[rw, cols], R=RA, W=[p1b])
                            tt_(Lm, p1[:, 0:128], m_sl, ALU.mult, [p1b, csb], [Lmb])
                            p2, p2b = self.bank()
                            self.mm(p2[:, 0:128], Bt[rw, cols], At[rw, cols], R=RA, W=[p2b])
                            self.mm(p2[:, 128:256], Bt[rw, cols], Rt[rw, cols], R=RA, W=[p2b])
                            tt_(NMr, p2[:, 0:256], m_up2, ALU.mult, [p2b, csb], [NMrb])
                            p3, p3b = self.bank()
                            self.mm(p3[:, 0:128], Kt[rw, cols], At[rw, cols], R=RA, W=[p3b])
                            self.mm(p3[:, 128:256], Kt[rw, cols], Rt[rw, cols], R=RA, W=[p3b])
                            tt_(KMr, p3[:, 0:256], m_up2, ALU.mult, [p3b, csb], [KMrb])
                            MakT, MrkT, MrbT = KMr[:, 0:128], KMr[:, 128:256], NMr[:, 128:256]
                            if 'u1' in SK:
                                continue
                            qi_ = 0
                            tt_(Qm[0], NMr[:, 0:128], ident, ALU.add, [NMrb, csb], [Qmb[0]])
                            px, pxb = self.bank()
                            self.mm(px[:, 0:128], Lm, NMr[:, 0:128], R=[Lmb, NMrb], W=[pxb])
                            self.mm(px[:, 128:256], NMr[:, 0:128], Lm, R=[Lmb, NMrb], W=[pxb])
                            cpA(LN[0], px[:, 0:256], [pxb], [LNb[0]])
                            li_ = 0
                            lvl = 2
                            while lvl <= int(os.environ.get('KLVL', '32')):
                                Nk, Lk = LN[li_][:, 0:128], LN[li_][:, 128:256]
                                px, pxb = self.bank()
                                self.mm(px[:, 0:128], Lk, Qm[qi_], R=[LNb[li_], Qmb[qi_]], W=[pxb])
                                KV = os.environ.get('KVAR', '')
                                if lvl < 32 and KV != 'v2':
                                    px2, px2b = self.bank()
                                    self.mm(px2[:, 0:128], Lk, Nk, R=[LNb[li_]], W=[px2b])
                                    self.mm(px2[:, 128:256], Nk, Lk, R=[LNb[li_]], W=[px2b])
                                if KV != 'v1':
                                    tt_(Qm[1 - qi_], px[:, 0:128], Qm[qi_], ALU.add, [pxb, Qmb[qi_]], [Qmb[1 - qi_]])
                                    qi_ = 1 - qi_
                                if lvl < 32 and KV != 'v2':
                                    cpA(LN[1 - li_], px2[:, 0:256], [px2b], [LNb[1 - li_]])
                                    li_ = 1 - li_
                                lvl *= 2
                                if 'nobar' not in SK:
                                    self.barrier3()
                            Q = Qm[qi_]; Qb_ = Qmb[qi_]
                            if 'u2' in SK:
                                continue
                            pm, pmb = self.bank()
                            self.mm(pm[:, 0:64], MakT, V_tm, R=[KMrb, tmb], W=[pmb])
                            cp_(MVs, pm[:, 0:64], [pmb], [MVsb])
                            if 'u2a' in SK:
                                continue
                            pw2, pw2b = self.bank()
                            self.mm(pw2[:, 0:64], Q, A_tm, R=[Qb_, tmb], W=[pw2b])
                            self.mm(pw2[:, 64:128], Q, MVs, R=[Qb_, MVsb], W=[pw2b])
                            cpA(WX, pw2[:, 0:128], [pw2b], [WXb])
                            if 'u2b' in SK:
                                continue
                            pgt, pgtb = self.bank()
                            self.mm(pgt[rw, 0:128], WX[:, 0:64], MrbT, R=[WXb, NMrb], W=[pgtb])
                            self.mm(pgt[rw, 128:256], V_tm, MrkT, start=True, stop=False, R=[tmb, KMrb], W=[pgtb])
                            self.mm(pgt[rw, 128:256], WX[:, 64:128], MrbT, start=False, stop=True, R=[WXb, NMrb], W=[pgtb])
                            if 'u2c' in SK:
                                continue
                            if 'e2' not in SK:
                                padd(GT[rw, :], pgt[rw, 0:128], Rt[rw, cols], [pgtb, Sb["Rt"]], [GTb], rw)
                            if 'e1' not in SK:
                                cpA(YlT[rw, :], pgt[rw, 128:256], [pgtb], [YlTb])
                            Pb_ = Pstb[hp][e]
                            if 'nobar' not in SK:
                                self.barrier3()
                            if 'u3' in SK:
                                continue
                            for c in range(2):
                                cc = slice(c * 64, (c + 1) * 64)
                                py, pyb = self.bank()
                                KMM = os.environ.get('KMM', '1234')
                                if '1' in KMM:
                                    self.mm(py[rw, 0:64], Pst[rw, hp, :], GT[rw, cc], R=[Pb_, GTb], W=[pyb])
                                if '2' in KMM:
                                    self.mm(py[rw, 64:128], WX[cc, 0:64], Bh_tm[cc, :], R=[WXb, tmb], W=[pyb])
                                if '3' in KMM:
                                    self.mm(py[rw, 128:192], Kh_tm[cc, :], V_tm[cc, :], R=[tmb], W=[pyb])
                                if '4' in KMM:
                                    self.mm(py[rw, 192:256], Bh_tm[cc, :], WX[cc, 64:128], R=[tmb, WXb], W=[pyb])
                                padd(S_["yA"][rw, b * 128 + c * 64:b * 128 + (c + 1) * 64], py[rw, 0:64], YlT[rw, cc], [pyb, YlTb], [Sb["yA"]], rw)
                                if 'c1' in SK:
                                    continue
                                gc = b * 2 + c
                                i_ = pei[0] % 2; pei[0] += 1
                                self.act(pev[i_][rw, 0:64], py[rw, 64:128], AF.Copy, R=[pyb], W=[pevb[i_]])
                                stt_(FTs[rw, :], ident[rw, r0:r0 + 64], EC[rw, gc:gc + 1], pev[i_][rw, 0:64], ALU.mult, ALU.add, [csb, ECb, pevb[i_]], [FTsb])
                                if 'c2' in SK:
                                    continue
                                cpA(Hs[rw, :], py[rw, 128:192], [pyb], [Hsb])
                                padd(Hs[rw, :], py[rw, 192:256], Hs[rw, :], [pyb], [Hsb], rw)
                                if 'c3' in SK:
                                    continue
                                pp_, ppb_ = self.bank()
                                self.mm(pp_[rw, 0:64], FTs[rw, :], Pst[rw, hp, :], R=[FTsb, Pb_], W=[ppb_])
                                padd(Pst[rw, hp, :], pp_[rw, 0:64], Hs[rw, :], [ppb_, Hsb], [Pb_], rw)
                                if 'nobar' not in SK:
                                    self.barrier3()
                    self.heavy = False
                    yA = S_["yA"]; yAb = Sb["yA"]
                    gnorm(yA, yAb, bo64, 64e-5, S_["E0"], Sb["E0"], S_["E1"], Sb["E1"])
                    lc = self.PO["ln"] + l * 8
                    ts_(yA, yA, pv[:, lc + hp:lc + hp + 1], pv[:, lc + 4 + hp:lc + 4 + hp + 1], ALU.mult, ALU.add, [pvb], [yAb])
                    tt_(yA, yA, S_["bon"], ALU.add, [Sb["bon"]], [yAb])
                    tt_(ya[:, hp, :], yA, S_["gg"], ALU.mult, [yAb, Sb["gg"]], [yab])
                self.barrier()
                if 'ret' in SK:
                    continue
                self.sb_off = scr0
                names = ["zq", "cosT", "sinS", "y0", "y1", "y2", "y3", "yv", "t1", "t2", "sil"]
                S_ = {n: A([128, 512]) for n in names}
                Sb = {n: Buf(n) for n in names}
                posi = A([128, 512], I32); posib = Buf("posi")
                ni = A([128, 512], I32); nib = Buf("ni")
                off_qrb = self.sb_off
                qrb = A([128, 4, 512], BF16); qrbb = Buf("qrb")
                qdb = A([128, 4, 512], BF16); qdbb = Buf("qdb")
                krb = A([128, 4, 512], BF16); krbb = Buf("krb")
                kr = A([128, 4, 512]); krb32 = Buf("kr")
                Vb = A([128, 4, 1024], BF16); Vbb = Buf("Vb")
                SbT = [A([128, 128], BF16) for _ in range(2)]; SbTb = [Buf("S0"), Buf("S1")]
                kd = [A([128, 64], BF16) for _ in range(2)]; kdb = [Buf("kd0"), Buf("kd1")]
                perm = cs[:, CO["perm"]:CO["perm"] + 128]
                self.dma(self.SP, [(posi, positions[s, tsl].partition_broadcast(128))], posib, True)
                cp_(S_["y0"], posi, [posib], [Sb["y0"]])
                self.op(D_, lambda: V.tensor_scalar_mul(S_["y0"], S_["y0"], cs[:, CO["invf"]:CO["invf"] + 1]), R=[csb], W=[Sb["y0"]])
                for which, dst in ((0, "sinS"), (1, "cosT")):
                    if which == 1:
                        self.op(D_, lambda: V.tensor_scalar_add(S_["y0"], S_["y0"], 0.25), W=[Sb["y0"]])
                    cp_(ni, S_["y0"], [Sb["y0"]], [nib])
                    cp_(S_["y1"], ni, [nib], [Sb["y1"]])
                    tt_(S_["y1"], S_["y0"], S_["y1"], ALU.subtract, [Sb["y0"]], [Sb["y1"]])
                    ts_(S_["y2"], S_["y1"], 0.5, None, ALU.is_gt, ALU.bypass, [Sb["y1"]], [Sb["y2"]])
                    tt_(S_["y1"], S_["y1"], S_["y2"], ALU.subtract, [Sb["y2"]], [Sb["y1"]])
                    ts_(S_["y2"], S_["y1"], -0.5, None, ALU.is_lt, ALU.bypass, [Sb["y1"]], [Sb["y2"]])
                    tt_(S_["y1"], S_["y1"], S_["y2"], ALU.add, [Sb["y2"]], [Sb["y1"]])
                    ts_(S_["y1"], S_["y1"], 0.5, -0.5, ALU.min, ALU.max, [], [Sb["y1"]])
                    if which == 0:
                        self.act(S_[dst], S_["y1"], AF.Sin, R=[Sb["y1"], csb], W=[Sb[dst]], scale=cs[:, CO["sinsc"]:CO["sinsc"] + 1])
                    else:
                        self.act(S_[dst], S_["y1"], AF.Sin, R=[Sb["y1"]], W=[Sb[dst]], scale=float(2 * np.pi))
                wload(wS[0], wSb[0], mw, 0, 8, 1792, 1024)
                wload(wS[1], wSb[1], mw, 0, 8, 2816, 1024)
                for ci in range(8):
                    pb, pbb = self.bank()
                    for k in range(NCH):
                        self.mm(pb, wS[0][:, k, ci * 128:(ci + 1) * 128], ut[:, k, :], start=(k == 0), stop=(k == 7), R=[wSb[0], utb], W=[pbb])
                    self.act(S_["zq"], pb, AF.Copy, R=[pbb], W=[Sb["zq"]])
                    ps2, ps2b = self.bank()
                    self.mm(ps2, perm, S_["zq"], R=[csb, Sb["zq"]], W=[ps2b])
                    tt_(S_["y2"], S_["zq"], S_["cosT"], ALU.mult, [Sb["zq"], Sb["cosT"]], [Sb["y2"]])
                    tt_(S_["y3"], ps2, S_["sinS"], ALU.mult, [ps2b, Sb["sinS"]], [Sb["y3"]])
                    if ci < 4:
                        tt_(qrb[:, ci, :], S_["y2"], S_["y3"], ALU.add, [Sb["y2"], Sb["y3"]], [qrbb])
                        tt_(S_["y2"], S_["y2"], S_["y3"], ALU.add, [Sb["y3"]], [Sb["y2"]])
                        tt_(qdb[:, ci, :], S_["y2"], cs[:, CO["qdec"] + ci * 512:CO["qdec"] + (ci + 1) * 512], ALU.mult, [Sb["y2"], csb], [qdbb])
                    else:
                        tt_(kr[:, ci - 4, :], S_["y2"], S_["y3"], ALU.add, [Sb["y2"], Sb["y3"]], [krb32])
                        self.act(krb[:, ci - 4, :], kr[:, ci - 4, :], AF.Copy, R=[krb32], W=[krbb])
                for tb in range(4):
                    for half in range(2):
                        pb, pbb = self.bank()
                        for k in range(NCH):
                            self.mm(pb, ut[:, k, tb * 128:(tb + 1) * 128], wS[1][:, k, half * 512:(half + 1) * 512], start=(k == 0), stop=(k == 7),
                                    R=[utb, wSb[1]], W=[pbb])
                        self.act(Vb[:, tb, half * 512:(half + 1) * 512], pb, AF.Copy, R=[pbb], W=[Vbb])
                wload(wS[0], wSb[0], mw, 0, 8, 3840, 1024)
                si_ = 0
                for h in range(0 if 'reth' in SK else 8):
                    hp, e = h // 2, h % 2
                    r0 = e * 64
                    rw = slice(r0, r0 + 64)
                    Rb_ = Rstb[hp][e]
                    self.rot = list(range(6))
                    py, pyb = self.ps[6 + h % 2], self.psb[6 + h % 2]
                    for cb in range(4):
                        cols = slice(cb * 128, (cb + 1) * 128)
                        i2 = si_ % 2; si_ += 1
                        p1, p1b = self.bank()
                        self.mm(p1[:, 0:128], krb[rw, hp, cols], qrb[rw, hp, cols], R=[krbb, qrbb], W=[p1b])
                        tt_(SbT[i2], p1[:, 0:128], cs[:, CO["DT"] + h * 128:CO["DT"] + (h + 1) * 128], ALU.mult, [p1b, csb], [SbTb[i2]])
                        p2, p2b = self.bank()
                        self.op(self.PE, lambda: nc.tensor.transpose(p2[:, 0:64], kr[rw, hp, cols], ident[rw, r0:r0 + 64]), R=[krb32, csb], W=[p2b])
                        self.act(kd[i2], p2[:, 0:64], AF.Copy, R=[p2b, csb], W=[kdb[i2]], scale=cs[:, CO["kdec"] + h:CO["kdec"] + h + 1])
                        self.mm(py[:, cols], Vb[:, cb, h * 128:(h + 1) * 128], SbT[i2], start=True, stop=False, R=[Vbb, SbTb[i2]], W=[pyb])
                        self.mm(py[:, cols], Rbf[rw, hp, :], qdb[rw, hp, cols], start=False, stop=True, R=[Rb_, qdbb], W=[pyb])
                        p3, p3b = self.bank()
                        self.mm(p3[rw, 0:128], kd[i2], Vb[:, cb, h * 128:(h + 1) * 128], R=[kdb[i2], Vbb], W=[p3b])
                        stt_(Rst[rw, hp, :], Rst[rw, hp, :], float(gam[h] ** 128), p3[rw, 0:128], ALU.mult, ALU.add, [p3b], [Rb_])
                        self.act(Rbf[rw, hp, :], Rst[rw, hp, :], AF.Copy, W=[Rb_])
                    self.act(S_["yv"], py, AF.Copy, R=[pyb], W=[Sb["yv"]])
                    gnorm(S_["yv"], Sb["yv"], on128, 1e-5, S_["t1"], Sb["t1"], S_["t2"], Sb["t2"])
                    pb, pbb = self.bank()
                    for k in range(NCH):
                        self.mm(pb, wS[0][:, k, h * 128:(h + 1) * 128], ut[:, k, :], start=(k == 0), stop=(k == 7), R=[wSb[0], utb], W=[pbb])
                    self.act(S_["sil"], pb, AF.Silu, R=[pbb], W=[Sb["sil"]])
                    gnc = self.PO["gn"] + l * NCH + h
                    stt_(yb[:, h, :], S_["yv"], pv[:, gnc:gnc + 1], S_["sil"], ALU.mult, ALU.mult, [Sb["yv"], pvb, Sb["sil"]], [ybb])
                self.rot = list(range(8))
                self.barrier()
                mg = self.alloc([128, 8, 512], BF16, at=off_qrb); mgb = Buf("mg")
                gA = S_["y0"]; gAb = Sb["y0"]
                gB = S_["y1"]; gBb = Sb["y1"]
                wload(wS[1], wSb[1], mw, 0, 8, 4864, 1024)
                wload(wS[0], wSb[0], mw, 0, 8, 5888, 1024)
                wA_ = Vb; wA_b = Buf("wA_")
                wload(wA_, wA_b, w_branch_a[li], 0, 4, 0, 1024)
                gbc = self.PO["gate_b"] + l * 2 * NCH
                for d in range(NCH):
                    dc = slice(d * 128, (d + 1) * 128)
                    pb, pbb = self.bank()
                    for k in range(NCH):
                        self.mm(pb, wS[1][:, k, dc], ut[:, k, :], start=(k == 0), stop=(k == 7), R=[wSb[1], utb], W=[pbb])
                    self.act(gA[:, :], pb, AF.Sigmoid, R=[pbb, pvb], W=[gAb], bias=pv[:, gbc + d:gbc + d + 1])
                    pb, pbb = self.bank()
                    for k in range(NCH):
                        self.mm(pb, wS[0][:, k, dc], ut[:, k, :], start=(k == 0), stop=(k == 7), R=[wSb[0], utb], W=[pbb])
                    self.act(gB[:, :], pb, AF.Sigmoid, R=[pbb, pvb], W=[gBb], bias=pv[:, gbc + NCH + d:gbc + NCH + d + 1])
                    pb, pbb = self.bank()
                    for k in range(4):
                        self.mm(pb, wA_[:, k, dc], ya[:, k, :], start=(k == 0), stop=(k == 3), R=[wA_b, yab], W=[pbb])
                    tt_(gA, gA, pb, ALU.mult, [pbb], [gAb])
                    S_["mgd"] = gA
                    Sb["mgd"] = gAb
                    S_.setdefault("pend", []).append(d)
                    cp_(S_["y2"] if d % 2 == 0 else S_["y3"], gB, [gBb], [Sb["y2"] if d % 2 == 0 else Sb["y3"]])
                    cp_(mg[:, d, :], gA, [gAb], [mgb])
                    cp_(kr[:, d % 4, :] if d < 4 else S_[["zq", "cosT", "sinS", "yv"][d - 4]], gB, [gBb], [krb32 if d < 4 else Sb[["zq", "cosT", "sinS", "yv"][d - 4]]])
                wload(wS[1], wSb[1], w_branch_b[li], 0, 8, 0, 1024)
                wload(wS[0], wSb[0], mix_w_out[li], 0, 8, 0, 1024)
                for d in range(NCH):
                    dc = slice(d * 128, (d + 1) * 128)
                    gsrc = kr[:, d % 4, :] if d < 4 else S_[["zq", "cosT", "sinS", "yv"][d - 4]]
                    gsb = krb32 if d < 4 else Sb[["zq", "cosT", "sinS", "yv"][d - 4]]
                    pb, pbb = self.bank()
                    for k in range(NCH):
                        self.mm(pb, wS[1][:, k, dc], yb[:, k, :], start=(k == 0), stop=(k == 7), R=[wSb[1], ybb], W=[pbb])
                    tt_(S_["t1"], gsrc, pb, ALU.mult, [gsb, pbb], [Sb["t1"]])
                    tt_(mg[:, d, :], mg[:, d, :], S_["t1"], ALU.add, [Sb["t1"]], [mgb])
                for d in range(NCH):
                    dc = slice(d * 128, (d + 1) * 128)
                    pb, pbb = self.bank()
                    for k in range(NCH):
                        self.mm(pb, wS[0][:, k, dc], mg[:, k, :], start=(k == 0), stop=(k == 7), R=[wSb[0], mgb], W=[pbb])
                    hsl = self.hT[:, d, tsl]
                    tt_(hsl, hsl, pb, ALU.add, [pbb], [self.hb[d][tt]])
                self.barrier()

        def load_seq(s):
            for blk in range(T // 128):
                tt = blk // 4
                self.dma(self.SP, [(stage, x[s, blk * 128:(blk + 1) * 128, :])], stageb, True)
                for half in range(2):
                    pb, pbb = self.bank()
                    for cc in range(4):
                        c = half * 4 + cc
                        self.tr(pb[:, cc * 128:(cc + 1) * 128], stage[:, c * 128:(c + 1) * 128], R=[stageb, self.csb], W=[pbb])
                    self.op(self.DVE, lambda pb=pb, half=half, blk=blk: V.tensor_copy(
                        self.hT[:, half * 4:half * 4 + 4, blk * 128:(blk + 1) * 128], pb.rearrange("p (c t) -> p c t", c=4)),
                        R=[pbb], W=[self.hb[half * 4 + cc][tt] for cc in range(4)])

        def store_seq(s):
            gcol = self.PO["final_norm"]
            yn = stmp
            for tt in range(NT):
                if self.final:
                    pb, pbb = self.bank()
                    for c in range(NCH):
                        i = sqi[0] % 2
                        sqi[0] += 1
                        self.act(sq[i], self.hT[:, c, tt * 512:(tt + 1) * 512], AF.Square, R=[self.hb[c][tt]], W=[sqb[i]])
                        self.mm(pb, self.onesb, sq[i], start=(c == 0), stop=(c == NCH - 1), R=[self.onesbb, sqb[i]], W=[pbb])
                    self.rsqrt_eps(rstd, pb, pbb, rstdb)
                    for c in range(NCH):
                        hsl = self.hT[:, c, tt * 512:(tt + 1) * 512]
                        self.op(self.DVE, lambda c=c, hsl=hsl: V.scalar_tensor_tensor(
                            hsl, hsl, self.pv[:, gcol + c:gcol + c + 1], rstd, ALU.mult, ALU.mult),
                            R=[self.pvb, rstdb], W=[self.hb[c][tt]])
                for b4 in range(4):
                    blk = tt * 4 + b4
                    banks = [self.bank(), self.bank()]
                    for c in range(NCH):
                        pb, pbb = banks[c // 4]
                        self.tr(pb[:, (c % 4) * 128:(c % 4 + 1) * 128], self.hT[:, c, blk * 128:(blk + 1) * 128],
                                R=[self.hb[c][tt], self.csb], W=[pbb])
                    for half in range(2):
                        pb, pbb = banks[half]
                        if half == 0:
                            self.op(self.DVE, lambda pb=pb: V.tensor_copy(stage[:, 0:512], pb), R=[pbb], W=[stageb])
                        else:
                            self.act(stage[:, 512:1024], pb, AF.Copy, R=[pbb], W=[stageb])
                    self.dma(self.SP, [(out[s, blk * 128:(blk + 1) * 128, :], stage)], stageb, False)

        for s in range(NSEQ):
            load_seq(s)
            self.barrier()
            for l in self.layers:
                if "ffn1" in self.parts:
                    ffn(l, 0)
                    self.barrier()
                if "mix" in self.parts:
                    mixer(s, l)
                    self.barrier()
                if "cross" in self.parts:
                    cross(s, l)
                    self.barrier()
                if "ffn2" in self.parts:
                    ffn(l, 1)
                    self.barrier()
            store_seq(s)
        self.barrier()
        return nc

    PO = {}
    CO = {}

    def pack_layout(self):
        off = 0
        for name, n in [("norm_g", NL * 4 * NCH), ("final_norm", NCH), ("gate_b", NL * 2 * NCH), ("shift_mu", NL * 14),
                        ("w0", NL * 4), ("a0", NL * 4), ("kk", NL * 4), ("ka", NL * 4), ("rk", NL * 4), ("ln", NL * 8),
                        ("gn", NL * NCH), ("mem_norm", NL * NCH)]:
            self.PO[name] = off
            off += n
        self.NPV = off
        off = 0
        for name, n in [("ident", 128), ("bo1", 128), ("bo64", 128), ("on128", 128), ("m_sl", 128), ("m_up2", 256), ("reset", 512),
                        ("DT", 1024), ("qdec", 2048), ("kdec", 8), ("invf", 1), ("sinsc", 1), ("perm", 128)]:
            self.CO[name] = off
            off += n
        self.NCST = off


WNAMES = ["ffn_w_in", "ffn_w_out", "mix_w_in", "w_branch_a", "w_branch_b", "mix_w_out", "cross_w_q", "cross_w_kv", "cross_w_o",
          "a_w_up", "a_a_up", "a_g_up"]


def _fm(v):
    v = np.asarray(v, np.float32)
    lead = int(np.prod(v.shape[:-1])) if v.ndim > 1 else 1
    n = v.shape[-1] // 128
    return np.ascontiguousarray(v.reshape(lead, n, 128).transpose(2, 0, 1).reshape(128, lead * n))


def make_host_arrays(b, inputs):
    pv = np.zeros((128, b.NPV), np.float32)
    pv[:, b.PO["norm_g"]:b.PO["norm_g"] + NL * 4 * NCH] = _fm(inputs["norm_g"])
    pv[:, b.PO["final_norm"]:b.PO["final_norm"] + NCH] = _fm(inputs["final_norm"])
    def put(name, arr):
        a = _fm(arr)
        pv[:, b.PO[name]:b.PO[name] + a.shape[1]] = a
    put("gate_b", inputs["mix_gate_b"]); put("shift_mu", inputs["shift_mu"])
    put("w0", inputs["a_w0"]); put("a0", inputs["a_a0"]); put("kk", inputs["a_k_k"]); put("ka", inputs["a_k_a"])
    put("rk", np.asarray(inputs["a_r_k"]).reshape(NL, 512)); put("ln", inputs["a_ln"]); put("gn", inputs["b_gn"]); put("mem_norm", inputs["mem_norm"])
    cs = np.zeros((128, b.NCST), np.float32)
    cs[:, b.CO["ident"]:b.CO["ident"] + 128] = np.eye(128, dtype=np.float32)
    CO = b.CO
    tt = np.arange(128)
    blk = (tt[:, None] // 64) == (tt[None, :] // 64)
    cs[:, CO["bo1"]:CO["bo1"] + 128] = blk.astype(np.float32)
    cs[:, CO["bo64"]:CO["bo64"] + 128] = blk.astype(np.float32) / 64.0
    cs[:, CO["on128"]:CO["on128"] + 128] = 1.0 / 128.0
    cs[:, CO["m_sl"]:CO["m_sl"] + 128] = (blk & (tt[:, None] > tt[None, :])).astype(np.float32)
    cs[:, CO["m_up2"]:CO["m_up2"] + 128] = (blk & (tt[:, None] < tt[None, :])).astype(np.float32)
    cs[:, CO["m_up2"] + 128:CO["m_up2"] + 256] = (blk & (tt[:, None] <= tt[None, :])).astype(np.float32)
    rm = np.ones(512, np.float32); rm[0::64] = 0.0
    cs[:, CO["reset"]:CO["reset"] + 512] = rm[None, :]
    gam = 1.0 - 2.0 ** (-5.0 - np.arange(8, dtype=np.float64))
    for h in range(8):
        dd = (tt[None, :] - tt[:, None]).astype(np.float64)
        cs[:, CO["DT"] + h * 128:CO["DT"] + (h + 1) * 128] = np.where(dd >= 0, gam[h] ** np.maximum(dd, 0), 0.0) * 0.125
        cs[:, CO["kdec"] + h] = (gam[h] ** (127.0 - tt)) * 0.125
    for hp in range(4):
        for e in range(2):
            q = gam[2 * hp + e] ** (np.arange(512) % 128 + 1.0)
            cs[e * 64:(e + 1) * 64, CO["qdec"] + hp * 512:CO["qdec"] + (hp + 1) * 512] = q[None, :]
    invf = (10000.0 ** (-(np.arange(128) % 32).astype(np.float32) / np.float32(32.0))).astype(np.float32)
    cs[:, CO["invf"]] = (invf.astype(np.float64) / (2 * np.pi)).astype(np.float32)
    cs[:, CO["sinsc"]] = np.where((tt % 64) < 32, -2 * np.pi, 2 * np.pi)
    pm = np.zeros((128, 128), np.float32)
    for p in range(128):
        pm[p ^ 32, p] = 1.0
    cs[:, CO["perm"]:CO["perm"] + 128] = pm
    return pv, cs


def run(inputs, T, nseq_per_core, ncores, layers, final=True, parts=("ffn1", "mix", "cross", "ffn2"), trace=False):
    b = Builder(T, nseq_per_core, layers, final, parts)
    b.pack_layout()
    nc = b.build()
    pv, cs = make_host_arrays(b, inputs)
    in_maps = []
    wsel = {nm: np.ascontiguousarray(np.asarray(inputs[nm])[list(layers)]) for nm in WNAMES}
    for i in range(ncores):
        sl = slice(i * nseq_per_core, (i + 1) * nseq_per_core)
        m = {"x": np.ascontiguousarray(inputs["x"][sl, :T]), "pvec": pv, "cst": cs,
             "mem": np.ascontiguousarray(inputs["mem"][sl]), "positions": np.ascontiguousarray(inputs["positions"][sl, :T]).astype(np.int32)}
        for nm in WNAMES:
            m[nm] = wsel[nm]
        in_maps.append(m)
    res = run_bass_kernel_spmd(nc, in_maps, core_ids=list(range(ncores)), trace=trace)
    outs = np.concatenate([r["out"] for r in res.results], axis=0)
    return outs, res


def kernel(**inputs):
    inputs = {k: np.asarray(v) for k, v in inputs.items()}
    out, _ = run(inputs, 2048, 4, 8, list(range(NL)))
    return out.astype(np.float32)
```
